# Optimizing a Trainium2 kernel written in Bass

```python
import jax, jax.numpy as jnp
from jax import lax
import numpy as np

D_MODEL = 1024
BATCH = 2
SEQ = 8192
DEPTH = 4

GLA_HEADS = 4
GLA_HEAD_K = 64
GLA_HEAD_V = 128
GLA_DK = GLA_HEADS * GLA_HEAD_K
GLA_DV = GLA_HEADS * GLA_HEAD_V
GLA_GATE_RANK = 16
GLA_GATE_TAU = 16.0
GLA_CHUNK = 64
GLA_NORM_EPS = 1e-5
RWKV_HEADS = 8
RWKV_HEAD = 64
RWKV_DIM = RWKV_HEADS * RWKV_HEAD
RWKV_DECAY_RANK = 64
RWKV_ICLR_RANK = 64
RWKV_GATE_RANK = 128
RWKV_GN_EPS = 64e-5
ATT_HEADS = 8
ATT_HEAD = 64
ATT_DIM = ATT_HEADS * ATT_HEAD
DILATED_PATTERNS = ((128, 1), (512, 4), (2048, 16))
ATT_BLOCK = 128
N_BRANCH = 3
FFN_HIDDEN = -(-8 * D_MODEL // (3 * 256)) * 256
LN_EPS = 1e-5
DEEPNORM_ALPHA = (2 * DEPTH) ** 0.25
DEEPNORM_BETA = (8 * DEPTH) ** -0.25

GLA_WIDTHS = (GLA_DK, GLA_DK, GLA_DV, GLA_GATE_RANK, GLA_DV)
RWKV_WIDTHS = (RWKV_DIM, RWKV_DIM, RWKV_DIM, RWKV_DECAY_RANK, RWKV_ICLR_RANK, RWKV_GATE_RANK)
ATT_WIDTHS = (ATT_DIM, ATT_DIM, ATT_DIM)
RWKV_IN = sum(RWKV_WIDTHS)
GROUP_WIDTHS = (sum(GLA_WIDTHS), RWKV_IN, sum(ATT_WIDTHS), N_BRANCH * D_MODEL)
D_IN = sum(GROUP_WIDTHS)

kernel_name = "hybrid_gla_rwkv7_dilated_deepnorm_adaln"


def _split(t, widths):
    return jnp.split(t, np.cumsum(widths)[:-1].tolist(), axis=-1)


def _split_heads(t, n_heads):
    b, s, _ = t.shape
    return t.reshape(b, s, n_heads, -1).transpose(0, 2, 1, 3)


def _merge_heads(t):
    b, h, s, d = t.shape
    return t.transpose(0, 2, 1, 3).reshape(b, s, h * d)


def layer_norm(x, g, b):
    xf = x.astype(jnp.float32)
    mu = jnp.mean(xf, -1, keepdims=True)
    var = jnp.mean(jnp.square(xf - mu), -1, keepdims=True)
    return ((xf - mu) * lax.rsqrt(var + LN_EPS) * g + b).astype(x.dtype)


def token_shift(p, mu):
    prev = jnp.pad(p, ((0, 0), (1, 0), (0, 0)))[:, :-1]
    return p + (prev - p) * mu


def alibi_slopes(n_heads):
    return 2.0 ** (-8.0 * jnp.arange(1, n_heads + 1, dtype=jnp.float32) / n_heads)


def gla_mixer(q, k, v, a_lo, og, w_alpha, b_alpha, norm_g):
    f32 = jnp.float32
    b_, s_, _ = q.shape
    log_a = jax.nn.log_sigmoid((a_lo @ w_alpha + b_alpha).astype(f32)) / GLA_GATE_TAU
    nc = s_ // GLA_CHUNK

    def chunk(t):
        t = _split_heads(t.astype(f32), GLA_HEADS)
        return t.reshape(b_, GLA_HEADS, nc, GLA_CHUNK, t.shape[-1])

    q, k, v, log_a = chunk(q) * GLA_HEAD_K ** -0.5, chunk(k), chunk(v), chunk(log_a)
    cum = jnp.cumsum(log_a, axis=3)
    cum_last = cum[:, :, :, -1:, :]
    q_e = q * jnp.exp(cum)
    k_e = k * jnp.exp(-cum)
    causal = jnp.tril(jnp.ones((GLA_CHUNK, GLA_CHUNK), bool))
    att = jnp.where(causal, jnp.einsum('bhnck,bhnsk->bhncs', q_e, k_e), 0.0)
    o = jnp.einsum('bhncs,bhnsv->bhncv', att, v)
    d_state = jnp.einsum('bhnck,bhncv->bhnkv', k * jnp.exp(cum_last - cum), v)
    decay = jnp.exp(cum_last[:, :, :, 0, :])

    def step(state, inp):
        dec, ds = inp
        return dec[..., None] * state + ds, state

    state0 = jnp.zeros((b_, GLA_HEADS, GLA_HEAD_K, GLA_HEAD_V), f32)
    _, s_prev = lax.scan(step, state0, (jnp.moveaxis(decay, 2, 0), jnp.moveaxis(d_state, 2, 0)))
    s_prev = jnp.moveaxis(s_prev, 0, 2)
    o = o + jnp.einsum('bhnck,bhnkv->bhncv', q_e, s_prev)
    o = o.reshape(b_, GLA_HEADS, s_, GLA_HEAD_V)
    o = o * lax.rsqrt(jnp.mean(o * o, -1, keepdims=True) + GLA_NORM_EPS) * norm_g
    return _merge_heads(o) * jax.nn.silu(og.astype(f32))


def rwkv7_mixer(r, k, v, w_lo, a_lo, g_lo, w0, w_up, a0, a_up, g_up, k_k, k_a, r_k, gn_g, gn_b):
    f32 = jnp.float32
    b_, s_, _ = r.shape
    r, k, v, w_lo, a_lo, g_lo = (t.astype(f32) for t in (r, k, v, w_lo, a_lo, g_lo))
    w_log = -jax.nn.softplus(-(w0 + jnp.tanh(w_lo) @ w_up)) - 0.5
    decay = jnp.exp(-jnp.exp(w_log))
    a = jax.nn.sigmoid(a0 + a_lo @ a_up)
    g = jax.nn.sigmoid(g_lo) @ g_up
    kk = k * k_k
    k = k * (1.0 + (a - 1.0) * k_a)
    hd = lambda t: t.reshape(b_, s_, RWKV_HEADS, RWKV_HEAD)
    r, k, v, kk, a, decay = map(hd, (r, k, v, kk, a, decay))
    kk = kk / jnp.maximum(jnp.sqrt(jnp.sum(kk * kk, -1, keepdims=True)), 1e-12)

    def step(state, inp):
        r_t, w_t, k_t, v_t, kk_t, b_t = inp
        sa = jnp.einsum('bhvk,bhk->bhv', state, -kk_t)
        state = state * w_t[:, :, None, :] + sa[..., None] * b_t[:, :, None, :] + v_t[..., None] * k_t[:, :, None, :]
        return state, jnp.einsum('bhvk,bhk->bhv', state, r_t)

    tm = lambda t: jnp.moveaxis(t, 1, 0)
    state0 = jnp.zeros((b_, RWKV_HEADS, RWKV_HEAD, RWKV_HEAD), f32)
    _, y = lax.scan(step, state0, (tm(r), tm(decay), tm(k), tm(v), tm(kk), tm(kk * a)))
    y = jnp.moveaxis(y, 0, 1)
    mu = jnp.mean(y, -1, keepdims=True)
    var = jnp.mean(jnp.square(y - mu), -1, keepdims=True)
    y = (y - mu) * lax.rsqrt(var + RWKV_GN_EPS) * gn_g.reshape(RWKV_HEADS, RWKV_HEAD) + gn_b.reshape(RWKV_HEADS, RWKV_HEAD)
    y = y + jnp.sum(r * k * r_k, -1, keepdims=True) * v
    return y.reshape(b_, s_, RWKV_DIM) * g


def dilated_pattern(q, k, v, window, dilation):
    b_, h_, s_, dh = q.shape
    span = window // dilation
    unit = dilation * ATT_BLOCK
    s_pad = -(-s_ // unit) * unit
    n_sub = s_pad // dilation
    nb = n_sub // ATT_BLOCK

    def to_blocks(t):
        t = jnp.pad(t, ((0, 0), (0, 0), (0, s_pad - s_), (0, 0)))
        t = t.reshape(b_, h_, n_sub, dilation, dh).transpose(0, 1, 3, 2, 4)
        return t.reshape(b_, h_, dilation, nb, ATT_BLOCK, dh)

    def with_prev(t):
        prev = jnp.pad(t, ((0, 0), (0, 0), (0, 0), (1, 0), (0, 0), (0, 0)))[:, :, :, :-1]
        return jnp.concatenate([prev, t], axis=4)

    qb, kb, vb = to_blocks(q), to_blocks(k), to_blocks(v)
    kc, vc = with_prev(kb), with_prev(vb)
    s = jnp.einsum('bhrnqd,bhrnkd->bhrnqk', qb, kc) * dh ** -0.5
    key_idx = jnp.arange(2 * ATT_BLOCK)
    steps = jnp.arange(ATT_BLOCK)[:, None] + ATT_BLOCK - key_idx[None, :]
    blk = jnp.arange(nb)[:, None, None]
    valid = (steps >= 0) & (steps <= span) & ((blk > 0) | (key_idx >= ATT_BLOCK)[None, None, :])
    bias = -alibi_slopes(h_)[:, None, None, None, None] * (steps * dilation).astype(jnp.float32)
    s = jnp.where(valid, s + bias, -jnp.inf)
    m = jnp.max(s, -1, keepdims=True)
    p = jnp.exp(s - m)
    den = jnp.sum(p, -1, keepdims=True)
    o = jnp.einsum('bhrnqk,bhrnkd->bhrnqd', p, vc) / den
    lse = (m + jnp.log(den))[..., 0]

    def from_blocks(t):
        tail = t.shape[5:]
        t = t.reshape(b_, h_, dilation, n_sub, *tail)
        t = jnp.moveaxis(t, 2, 3).reshape(b_, h_, s_pad, *tail)
        return t[:, :, :s_]

    return from_blocks(o), from_blocks(lse)


def dilated_attention(q, k, v):
    q, k, v = (_split_heads(t.astype(jnp.float32), ATT_HEADS) for t in (q, k, v))
    res = [dilated_pattern(q, k, v, w, d) for (w, d) in DILATED_PATTERNS]
    outs = jnp.stack([o for o, _ in res])
    lses = jnp.stack([l for _, l in res])
    wts = jax.nn.softmax(lses, axis=0)
    return _merge_heads(jnp.sum(wts[..., None] * outs, axis=0))


def hybrid_mixer(u, w_in, gla_w_alpha, gla_b_alpha, gla_norm_g, rwkv_mu, rwkv_w0, rwkv_w_up,
                 rwkv_a0, rwkv_a_up, rwkv_g_up, rwkv_k_k, rwkv_k_a, rwkv_r_k, rwkv_gn_g, rwkv_gn_b,
                 w_branch, w_out):
    b_, s_, _ = u.shape
    p = u @ w_in
    gla_p, rwkv_p, att_p, gate_p = _split(p, GROUP_WIDTHS)
    gq, gk, gv, ga, gg = _split(gla_p, GLA_WIDTHS)
    rr, rk, rv, rw, ra, rg = _split(token_shift(rwkv_p, rwkv_mu), RWKV_WIDTHS)
    aq, ak, av = _split(att_p, ATT_WIDTHS)
    o_a = gla_mixer(gq, gk, gv, ga, gg, gla_w_alpha, gla_b_alpha, gla_norm_g)
    o_b = rwkv7_mixer(rr, rk, rv, rw, ra, rg, rwkv_w0, rwkv_w_up, rwkv_a0, rwkv_a_up, rwkv_g_up,
                      rwkv_k_k, rwkv_k_a, rwkv_r_k, rwkv_gn_g, rwkv_gn_b)
    o_c = dilated_attention(aq, ak, av)
    branches = jnp.stack([o_a, o_b, o_c], axis=2).astype(u.dtype)
    proj = jnp.einsum('bsnc,ncd->bsnd', branches, w_branch)
    gates = jax.nn.sigmoid(gate_p.astype(jnp.float32)).reshape(b_, s_, N_BRANCH, D_MODEL)
    merged = jnp.sum(gates * proj, axis=2).astype(u.dtype)
    return merged @ w_out


def swiglu(u, w1, w2):
    gate, up = jnp.split(u @ w1, 2, axis=-1)
    return (jax.nn.silu(gate) * up) @ w2


def setup_inputs(seed: int = 0) -> dict:
    key = jax.random.key(seed)
    ks = iter(jax.random.split(key, 40))
    f32 = jnp.float32
    L = DEPTH
    nrm = lambda shape, scale: jax.random.normal(next(ks), shape, f32) * scale
    uni = lambda shape, lo, hi: jax.random.uniform(next(ks), shape, f32, lo, hi)
    return {
        "x": nrm((BATCH, SEQ, D_MODEL), 1.0),
        "c": nrm((BATCH, D_MODEL), 1.0),
        "w_ada": nrm((L, D_MODEL, 6 * D_MODEL), 0.1 * D_MODEL ** -0.5),
        "b_ada": nrm((L, 6 * D_MODEL), 0.01),
        "w_in": nrm((L, D_MODEL, D_IN), D_MODEL ** -0.5),
        "gla_w_alpha": nrm((L, GLA_GATE_RANK, GLA_DK), GLA_GATE_RANK ** -0.5),
        "gla_b_alpha": nrm((L, GLA_DK), 0.1),
        "gla_norm_g": 1.0 + nrm((L, GLA_HEAD_V), 0.02),
        "rwkv_mu": uni((L, RWKV_IN), 0.0, 1.0),
        "rwkv_w0": uni((L, RWKV_DIM), -6.0, 1.0),
        "rwkv_w_up": nrm((L, RWKV_DECAY_RANK, RWKV_DIM), 0.1 * RWKV_DECAY_RANK ** -0.5),
        "rwkv_a0": nrm((L, RWKV_DIM), 0.1),
        "rwkv_a_up": nrm((L, RWKV_ICLR_RANK, RWKV_DIM), 0.1 * RWKV_ICLR_RANK ** -0.5),
        "rwkv_g_up": nrm((L, RWKV_GATE_RANK, RWKV_DIM), RWKV_GATE_RANK ** -0.5),
        "rwkv_k_k": 0.85 + nrm((L, RWKV_DIM), 0.02),
        "rwkv_k_a": 1.0 + nrm((L, RWKV_DIM), 0.02),
        "rwkv_r_k": nrm((L, RWKV_HEADS, RWKV_HEAD), 0.1),
        "rwkv_gn_g": 1.0 + nrm((L, RWKV_DIM), 0.02),
        "rwkv_gn_b": nrm((L, RWKV_DIM), 0.01),
        "w_branch": nrm((L, N_BRANCH, ATT_DIM, D_MODEL), ATT_DIM ** -0.5),
        "w_out": nrm((L, D_MODEL, D_MODEL), DEEPNORM_BETA * D_MODEL ** -0.5),
        "ln1_g": 1.0 + nrm((L, D_MODEL), 0.02),
        "ln1_b": nrm((L, D_MODEL), 0.01),
        "ffn_w1": nrm((L, D_MODEL, 2 * FFN_HIDDEN), D_MODEL ** -0.5),
        "ffn_w2": nrm((L, FFN_HIDDEN, D_MODEL), DEEPNORM_BETA * FFN_HIDDEN ** -0.5),
        "ln2_g": 1.0 + nrm((L, D_MODEL), 0.02),
        "ln2_b": nrm((L, D_MODEL), 0.01),
    }


def reference(x, c, w_ada, b_ada, w_in, gla_w_alpha, gla_b_alpha, gla_norm_g, rwkv_mu, rwkv_w0,
              rwkv_w_up, rwkv_a0, rwkv_a_up, rwkv_g_up, rwkv_k_k, rwkv_k_a, rwkv_r_k, rwkv_gn_g,
              rwkv_gn_b, w_branch, w_out, ln1_g, ln1_b, ffn_w1, ffn_w2, ln2_g, ln2_b):
    for l in range(DEPTH):
        mod = jax.nn.silu(c) @ w_ada[l] + b_ada[l]
        sh1, sc1, g1, sh2, sc2, g2 = jnp.split(mod[:, None, :], 6, axis=-1)
        u = x * (1.0 + sc1) + sh1
        h = hybrid_mixer(u, w_in[l], gla_w_alpha[l], gla_b_alpha[l], gla_norm_g[l], rwkv_mu[l],
                         rwkv_w0[l], rwkv_w_up[l], rwkv_a0[l], rwkv_a_up[l], rwkv_g_up[l],
                         rwkv_k_k[l], rwkv_k_a[l], rwkv_r_k[l], rwkv_gn_g[l], rwkv_gn_b[l],
                         w_branch[l], w_out[l])
        x = layer_norm(DEEPNORM_ALPHA * x + (1.0 + g1) * h, ln1_g[l], ln1_b[l])
        u = x * (1.0 + sc2) + sh2
        h = swiglu(u, ffn_w1[l], ffn_w2[l])
        x = layer_norm(DEEPNORM_ALPHA * x + (1.0 + g2) * h, ln2_g[l], ln2_b[l])
    return x
```

```python
import numpy as np
import concourse.bass as bass
import concourse.mybir as mybir
from concourse.bass_utils import run_bass_kernel_spmd

F32 = mybir.dt.float32
BF16 = mybir.dt.bfloat16
AF = mybir.ActivationFunctionType
ALU = mybir.AluOpType
AX = mybir.AxisListType

EPOCH = 24000


def _box(ap):
    shape = tuple(ap.tensor.shape)
    if type(ap.tensor).__name__.startswith("PSum"):
        return tuple((0, s - 1) for s in shape)
    off = int(ap.offset)
    hi = off
    for step, cnt in ap.ap:
        if cnt > 1:
            hi += step * (cnt - 1)
    lo_idx = np.unravel_index(off, shape)
    hi_idx = np.unravel_index(hi, shape)
    return tuple((int(a), int(b)) for a, b in zip(lo_idx, hi_idx))


def _overlap(b1, b2):
    for (a0, a1), (c0, c1) in zip(b1, b2):
        if a1 < c0 or c1 < a0:
            return False
    return True


def _covers(b1, b2):
    for (a0, a1), (c0, c1) in zip(b1, b2):
        if c0 < a0 or c1 > a1:
            return False
    return True


class _Ins:
    __slots__ = ("eng", "fn", "dmakey", "idx", "waits", "signal", "sigidx", "dmacount")

    def __init__(self, eng, fn, dmakey):
        self.eng = eng
        self.fn = fn
        self.dmakey = dmakey
        self.waits = {}
        self.signal = False
        self.sigidx = None
        self.dmacount = None


class Prog:
    ENGS = ("pe", "act", "dve", "pool", "sp")

    def __init__(self, nc):
        self.nc = nc
        self.ins = []
        self.acc = {}
        self.dma_counts = {}
        self.sems = {}
        self._semctx = []

    def _record(self, eng, fn, reads, writes, dmakey=None):
        ins = _Ins(eng, fn, dmakey)
        ins.idx = len(self.ins)
        if dmakey is not None:
            c = self.dma_counts.get(dmakey, 0) + 1
            self.dma_counts[dmakey] = c
            ins.dmacount = c
        deps = set()
        writes = list(writes) + [ap for ap in reads if ap is not None and hasattr(ap, "tensor")
                                 and type(ap.tensor).__name__.startswith("PSum")]
        for ap in reads:
            if ap is None or not hasattr(ap, "tensor"):
                continue
            if type(ap.tensor).__name__.startswith("PSum"):
                continue
            name = ap.tensor.name
            box = _box(ap)
            lst = self.acc.setdefault(name, [])
            for (b, j, w) in lst:
                if w and _overlap(b, box):
                    deps.add(j)
            if dmakey is None:
                lst[:] = [r for r in lst if not ((not r[2]) and r[0] == box and r[1] < len(self.ins)
                                                 and self.ins[r[1]].eng == eng
                                                 and self.ins[r[1]].dmakey is None)]
            lst.append((box, ins.idx, False))
        for ap in writes:
            name = ap.tensor.name
            box = _box(ap)
            lst = self.acc.setdefault(name, [])
            keep = []
            for rec in lst:
                b, j, w = rec
                if _overlap(b, box):
                    deps.add(j)
                    if _covers(box, b):
                        continue
                keep.append(rec)
            keep.append((box, ins.idx, True))
            self.acc[name] = keep
        deps.discard(ins.idx)
        for j in deps:
            J = self.ins[j]
            if J.dmakey is not None:
                dom = ("dma", J.dmakey)
                cnt = self.dma_counts[J.dmakey]
                if J is ins:
                    continue
                if ins.dmakey == J.dmakey:
                    cnt = ins.dmacount - 1
                    if cnt <= 0:
                        continue
                ins.waits[dom] = max(ins.waits.get(dom, 0), cnt)
            else:
                if J.eng == eng and dmakey is None and eng == "pe":
                    continue
                if J.eng == eng and dmakey is not None:
                    pass
                J.signal = True
                dom = ("eng", J.eng)
                ins.waits[dom] = max(ins.waits.get(dom, 0), -1)
                ins.waits.setdefault(("dep", j), 1)
        self.ins.append(ins)
        return ins

    def mm(self, out, lhsT, rhs, start=True, stop=True, **kw):
        return self._record("pe", lambda e: e.matmul(out, lhsT, rhs, start=start, stop=stop, **kw),
                            [lhsT, rhs], [out])

    def transpose(self, out, in_, ident):
        return self._record("pe", lambda e: e.transpose(out, in_, ident), [in_, ident], [out])

    def act(self, out, in_, func, bias=None, scale=None, accum_out=None, eng="act"):
        kw = {}
        rd = [in_]
        wr = [out]
        if bias is not None:
            kw["bias"] = bias
            rd.append(bias)
        if scale is not None:
            kw["scale"] = scale
            rd.append(scale)
        if accum_out is not None:
            kw["accum_out"] = accum_out
            wr.append(accum_out)
        return self._record(eng, lambda e: e.activation(out, in_, func, **kw), rd, wr)

    def tt(self, out, in0, in1, op, eng="dve"):
        return self._record(eng, lambda e: e.tensor_tensor(out, in0, in1, op), [in0, in1], [out])

    def ts(self, out, in0, s1, s2, op0, op1=None, eng="dve", accum_out=None):
        kw = {}
        wr = [out]
        if op1 is not None:
            kw["op1"] = op1
        if accum_out is not None:
            kw["accum_out"] = accum_out
            wr.append(accum_out)
        return self._record(eng, lambda e: e.tensor_scalar(out, in0, s1, s2, op0, **kw), [in0, s1, s2], wr)

    def stt(self, out, in0, scalar, in1, op0, op1, eng="dve"):
        return self._record(eng, lambda e: e.scalar_tensor_tensor(out, in0, scalar, in1, op0, op1),
                            [in0, scalar, in1], [out])

    def copy(self, out, in_, eng="dve"):
        if eng == "act":
            return self._record(eng, lambda e: e.activation(out, in_, AF.Copy), [in_], [out])
        return self._record(eng, lambda e: e.tensor_copy(out, in_), [in_], [out])

    def memset(self, ap, val, eng="pool"):
        return self._record(eng, lambda e: e.memset(ap, val), [], [ap])

    def reduce(self, out, in_, op, axis=AX.X, eng="dve"):
        return self._record(eng, lambda e: e.tensor_reduce(out, in_, axis, op), [in_], [out])

    def recip(self, out, in_, eng="dve"):
        return self._record(eng, lambda e: e.reciprocal(out, in_), [in_], [out])

    def bn_stats(self, out, in_):
        return self._record("dve", lambda e: e.bn_stats(out, in_), [in_], [out])

    def bn_aggr(self, out, in_):
        return self._record("dve", lambda e: e.bn_aggr(out, in_), [in_], [out])

    def dma(self, out, in_, key, q="sp"):
        return self._record(q, lambda e: e.dma_start(out=out, in_=in_), [in_], [out], dmakey="%s_%s" % (key, q))

    def custom(self, eng, fn, reads, writes):
        return self._record(eng, fn, reads, writes)

    def barrier(self):
        last = {}
        for I in self.ins:
            if I.dmakey is None and I.fn is not None:
                last[I.eng] = I
        dmac = dict(self.dma_counts)
        for e in self.ENGS:
            ins = _Ins(e, None, None)
            ins.idx = len(self.ins)
            for e2, J in last.items():
                if e2 == e:
                    continue
                J.signal = True
                ins.waits[("dep", J.idx)] = 1
            for key, cnt in dmac.items():
                ins.waits[("dma", key)] = cnt
            self.ins.append(ins)
        self.acc = {}

    def _sem_for(self, dom, count, step):
        ep = (count - 1) // EPOCH
        h = self._get_sem(dom, ep)
        return ep, h, (count - ep * EPOCH) * step

    def _get_sem(self, dom, ep):
        k = (dom, ep)
        if k not in self.sems:
            name = "s_%s_%d" % ("_".join(str(x) for x in dom), ep)
            ctx = self.nc.semaphore(name)
            h = ctx.__enter__()
            self._semctx.append(ctx)
            self.sems[k] = h
        return self.sems[k]

    def finalize(self, final_wait_keys=()):
        nc = self.nc
        sigcount = {e: 0 for e in self.ENGS}
        for I in self.ins:
            if I.dmakey is None and I.signal:
                sigcount[I.eng] += 1
                I.sigidx = sigcount[I.eng]
        per_eng = {e: [] for e in self.ENGS}
        waited = {e: {} for e in self.ENGS}
        plan = []
        for I in self.ins:
            need = {}
            for k, v in I.waits.items():
                if k[0] == "dep":
                    J = self.ins[k[1]]
                    dom = ("eng", J.eng)
                    need[dom] = max(need.get(dom, 0), J.sigidx)
                elif k[0] == "dma":
                    need[k] = max(need.get(k, 0), v)
            wl = []
            w = waited[I.eng]
            for dom, cnt in need.items():
                if w.get(dom, 0) >= cnt:
                    continue
                step = 16 if dom[0] == "dma" else 1
                ep, h, val = self._sem_for(dom, cnt, step)
                if dom[0] == "dma":
                    prev = w.get(dom, 0)
                    pe_ = (prev - 1) // EPOCH if prev > 0 else -1
                    for e2 in range(max(pe_, 0), ep):
                        wl.append((self._get_sem(dom, e2), EPOCH * 16))
                wl.append((h, val))
                w[dom] = cnt
            sig = None
            if I.dmakey is not None:
                dom = ("dma", I.dmakey)
                ep, h, _ = self._sem_for(dom, I.dmacount, 16)
                sig = (h, 16)
            elif I.signal:
                dom = ("eng", I.eng)
                ep, h, _ = self._sem_for(dom, I.sigidx, 1)
                sig = (h, 1)
            per_eng[I.eng].append((wl, I.fn, sig))
        fin = []
        for key, cnt in self.dma_counts.items():
            dom = ("dma", key)
            ep, h, val = self._sem_for(dom, cnt, 16)
            for e2 in range(0, ep):
                fin.append((self._get_sem(dom, e2), EPOCH * 16))
            fin.append((h, val))
        for e in ("pe", "act", "dve", "pool"):
            if sigcount[e] > 0:
                ep, h, val = self._sem_for(("eng", e), sigcount[e], 1)
                fin.append((h, val))

        def run(engname, e):
            for wl, fn, sig in per_eng[engname]:
                for (h, val) in wl:
                    e.wait_ge(h, val)
                if fn is None:
                    continue
                inst = fn(e)
                if sig is not None:
                    inst.then_inc(sig[0], sig[1])
            if engname == "sp":
                for (h, val) in fin:
                    e.wait_ge(h, val)

        with nc.Block() as block:
            @block.tensor
            def _(e):
                run("pe", e)

            @block.scalar
            def _(e):
                run("act", e)

            @block.vector
            def _(e):
                run("dve", e)

            @block.gpsimd
            def _(e):
                run("pool", e)

            @block.sync
            def _(e):
                run("sp", e)
        for ctx in reversed(self._semctx):
            ctx.__exit__(None, None, None)
        self.stats = {e: len(per_eng[e]) for e in self.ENGS}
        return self.stats


D_MODEL = 1024
SEQ = 8192
BATCH = 2
DEPTH = 4
NCORES = 8
FFN_HIDDEN = 2816
DEEPNORM_ALPHA = (2 * DEPTH) ** 0.25
GLA_OFF = 0
RWKV_OFF = 1552
ATT_OFF = 3344
GATE_OFF = 4880
D_IN = 7952
WDEC = -0.6065306597126334
NEG = -30000.0


def alibi_slopes_np(n):
    return (2.0 ** (-8.0 * np.arange(1, n + 1, dtype=np.float32) / n)).astype(np.float32)


def make_consts():
    t = np.arange(128)
    tp, tt_ = np.meshgrid(t, t, indexing="ij")
    c = np.zeros((128, 8, 128), np.float32)
    c[:, 0] = (tp <= tt_)
    c[:, 1] = (tp < tt_)
    c[:, 2] = 1.0
    c[:, 3] = (tp == tt_)
    c[:, 4] = (tp < tt_)
    c[:, 5] = (tp <= tt_)
    c[:, 6] = (tp > tt_)
    c[:, 7] = 0.0
    return c


def make_att_bias(g):
    slopes = alibi_slopes_np(8)
    out = np.zeros((128, 3, 2, 256), np.float32)
    j = np.arange(128)[:, None]
    i = np.arange(128)[None, :]
    for di, d in enumerate((1, 4, 16)):
        for h in range(2):
            sl = slopes[2 * g + h]
            steps_prev = i + 128 - j
            steps_cur = i - j
            bp = np.where((steps_prev >= 0) & (steps_prev <= 128), -sl * steps_prev * d, NEG)
            bc = np.where((steps_cur >= 0) & (steps_cur <= 128), -sl * steps_cur * d, NEG)
            out[:, di, h, 0:128] = bp
            out[:, di, h, 128:256] = bc
    return out


from contextlib import ExitStack


class Alloc:
    def __init__(self, nc):
        self.nc = nc
        self.stack = ExitStack()

    def sb(self, name, shape, dt):
        return self.stack.enter_context(self.nc.sbuf_tensor(name, list(shape), dt))

    def ps(self, name, shape, dt):
        return self.stack.enter_context(self.nc.psum_tensor(name, list(shape), dt))

    def close(self):
        self.stack.close()


class Rot:
    def __init__(self, items):
        self.items = list(items)
        self.i = 0

    def next(self):
        x = self.items[self.i % len(self.items)]
        self.i += 1
        return x


FM_COLS = dict(gq=(0, 64), gk=(64, 128), ga=(128, 144), rwa=(144, 272), rg=(272, 400),
               aq=(400, 528), ak=(528, 656), av=(656, 784))
NFM = 784
TM_COLS = dict(gtm=(0, 320), rkv=(320, 704))
NTM = 704


def mixer_weight_cols(g):
    ar = np.arange
    fm = np.concatenate([
        GLA_OFF + 64 * g + ar(64),
        GLA_OFF + 256 + 64 * g + ar(64),
        GLA_OFF + 1024 + ar(16),
        RWKV_OFF + 1536 + ar(64),
        RWKV_OFF + 1600 + ar(64),
        RWKV_OFF + 1664 + ar(128),
        ATT_OFF + 128 * g + ar(128),
        ATT_OFF + 512 + 128 * g + ar(128),
        ATT_OFF + 1024 + 128 * g + ar(128),
    ])
    tm = np.concatenate([
        GLA_OFF + 256 + 64 * g + ar(64),
        GLA_OFF + 512 + 128 * g + ar(128),
        GLA_OFF + 1040 + 128 * g + ar(128),
        RWKV_OFF + 128 * g + ar(128),
        RWKV_OFF + 512 + 128 * g + ar(128),
        RWKV_OFF + 1024 + 128 * g + ar(128),
    ])
    return fm, tm


def emit_attention(nc, P, S, uT_d, wfm_sb, cst, attb_d, outT_d):
    A = Alloc(nc)
    NU = S // 2048
    qTh = [A.sb("at_qT%d" % h, [64, 2048], BF16) for h in range(2)]
    kTh = [A.sb("at_kT%d" % h, [64, 2, 2048], BF16) for h in range(2)]
    vT = A.sb("at_vT", [128, 2048], BF16)
    sel65 = A.sb("at_sel65", [65, 64], F32)
    ub = [A.sb("at_u%d" % i, [128, 8, 512], BF16) for i in range(2)]
    attb = A.sb("at_bias", [128, 3, 2, 256], F32)
    identb = A.sb("at_identb", [128, 128], BF16)
    vaug = A.sb("at_vaug", [128, 2, 48, 2, 65], BF16)
    acc = A.sb("at_acc", [65, 2, 2048], F32)
    tmp = [A.sb("at_tmp%d" % i, [128, 2, 256], F32) for i in range(3)]
    pT = [A.sb("at_pT%d" % i, [128, 2, 256], BF16) for i in range(3)]
    rden = A.sb("at_rden", [65, 2, 512], F32)
    oT = [A.sb("at_oT%d" % i, [64, 2048], BF16) for i in range(2)]
    ps_a = [A.ps("at_pa%d" % i, [128, 512], F32) for i in range(2)]
    ps_s = [A.ps("at_ps%d" % i, [128, 2, 256], F32) for i in range(3)]
    ps_o = [A.ps("at_po%d" % i, [128, 2, 256], F32) for i in range(3)]

    P.dma(attb[:].rearrange("p a b c -> p (a b c)"), attb_d, key="at_c", q="sp")
    P.copy(identb[:], cst[:, 3, :], eng="dve")
    P.memset(vaug[:].rearrange("p a b c d -> p (a b c d)"), 1.0, eng="pool")
    P.memset(sel65[:], 0.0, eng="pool")
    P.memset(sel65[64:65, :], 1.0, eng="pool")
    P.memset(rden[:].rearrange("p a b -> p (a b)"), 0.0, eng="pool")
    uT_v = uT_d.rearrange("(k p) t -> p k t", p=128)
    evac = Rot(["act", "dve"])
    actr = 0
    bctr = 0

    def tile_id(di, d, r, n):
        return di * 16 + r * (16 // d) + n

    sctr = 0
    for U in range(NU):
        ring = U % 2
        base = U * 2048
        for tb in range(4):
            u = ub[bctr % 2]
            P.dma(u[:], uT_v[:, :, base + tb * 512:base + (tb + 1) * 512], key="at_u%d" % (bctr % 2), q="sp")
            bctr += 1
            lc = slice(tb * 512, (tb + 1) * 512)
            for nm in ("aq", "ak"):
                c0, c1 = FM_COLS[nm]
                for h in range(2):
                    pp = ps_a[actr % 2]
                    actr += 1
                    for k in range(8):
                        P.mm(pp[0:64, :], wfm_sb[:, k, c0 + 64 * h:c0 + 64 * h + 64], u[:, k, :],
                             start=(k == 0), stop=(k == 7))
                    dst = qTh[h][:, lc] if nm == "aq" else kTh[h][:, ring, lc]
                    P.copy(dst, pp[0:64, :], eng=evac.next())
            c0, c1 = FM_COLS["av"]
            pp = ps_a[actr % 2]
            actr += 1
            for k in range(8):
                P.mm(pp[:, :], wfm_sb[:, k, c0:c1], u[:, k, :], start=(k == 0), stop=(k == 7))
            P.copy(vT[:, lc], pp[:, :], eng=evac.next())
        tiles = []
        for di, d in enumerate((1, 4, 16)):
            for r in range(d):
                for n in range(16 // d):
                    tiles.append((tile_id(di, d, r, n), r + d * 128 * n, d))
        tiles.sort()
        for g4 in range(0, 48, 4):
            pt = ps_a[actr % 2]
            actr += 1
            for q in range(4):
                tid, l0, d = tiles[g4 + q]
                assert tid == g4 + q
                P.mm(pt[:, q * 128:(q + 1) * 128], vT[:, l0:l0 + d * 127 + 1:d], identb[:], start=True, stop=True)
            P.copy(vaug[:, ring, g4:g4 + 4, :, 0:64],
                   pt[:, :].rearrange("p (q h c) -> p q h c", q=4, h=2), eng=evac.next())
        for di, d in enumerate((1, 4, 16)):
            for r in range(d):
                for n in range(16 // d):
                    l0 = r + d * 128 * n
                    cur_sl = slice(l0, l0 + d * 127 + 1, d)
                    has_prev = not (U == 0 and n == 0)
                    if n > 0:
                        pring, ptile = ring, tile_id(di, d, r, n - 1)
                        p0 = l0 - d * 128
                    else:
                        pring, ptile = 1 - ring, tile_id(di, d, r, 16 // d - 1)
                        p0 = 2048 + l0 - d * 128
                    prev_sl = slice(p0, p0 + d * 127 + 1, d)
                    pss = ps_s[sctr % 3]
                    pso = ps_o[sctr % 3]
                    tb_ = tmp[sctr % 3]
                    pt_ = pT[sctr % 3]
                    sctr += 1
                    lo = 0 if has_prev else 128
                    for h in range(2):
                        if has_prev:
                            P.mm(pss[:, h, 0:128], kTh[h][:, pring, prev_sl], qTh[h][:, cur_sl])
                        P.mm(pss[:, h, 128:256], kTh[h][:, ring, cur_sl], qTh[h][:, cur_sl])
                    P.stt(tb_[:, :, lo:256], pss[:, :, lo:256], 0.125, attb[:, di, :, lo:256],
                          ALU.mult, ALU.add, eng="dve")
                    P.act(pt_[:, :, lo:256], tb_[:, :, lo:256], AF.Exp)
                    for h in range(2):
                        if has_prev:
                            P.mm(pso[0:65, h, 0:128], vaug[:, pring, ptile, h, :], pt_[:, h, 0:128],
                                 start=True, stop=False)
                        P.mm(pso[0:65, h, 0:128], vaug[:, ring, tile_id(di, d, r, n), h, :],
                             pt_[:, h, 128:256], start=(not has_prev), stop=True)
                    if di == 0:
                        P.copy(acc[:, :, cur_sl], pso[0:65, :, 0:128], eng="dve")
                    else:
                        P.tt(acc[:, :, cur_sl], acc[:, :, cur_sl], pso[0:65, :, 0:128], ALU.add, eng="dve")
        for h in range(2):
            ot = oT[h]
            for c in range(4):
                cs = slice(c * 512, (c + 1) * 512)
                P.recip(rden[64:65, h, :], acc[64:65, h, cs], eng="dve")
                pb = ps_a[actr % 2]
                actr += 1
                P.mm(pb[0:64, :], sel65[:, :], rden[:, h, :])
                P.tt(ot[:, cs], acc[0:64, h, cs], pb[0:64, :], ALU.mult, eng="dve")
            P.dma(outT_d[256 + 64 * h:256 + 64 * h + 64, base:base + 2048], ot[:], key="at_o%d" % h, q="sp")
    P.barrier()
    A.close()


PROW = dict(b_alpha=(0, 64), w0=(64, 192), a0=(192, 320))
NROW = 320
PBC = dict(norm_g=(0, 128), mu_rkv=(128, 512), k_k=(512, 640), k_a=(640, 768), gn_g=(768, 896),
           gn_b=(896, 1024), r_k=(1024, 1152))
NBC = 1152


def emit_gla(nc, P, S, uT_d, wfm_sb, wtm_sb, cst, prow, pbc, wsm, outT_d):
    A = Alloc(nc)
    NB = S // 512
    ub = [A.sb("gl_u%d" % i, [128, 8, 512], BF16) for i in range(2)]
    qT = [A.sb("gl_qT%d" % i, [64, 512], F32) for i in range(2)]
    kT = [A.sb("gl_kT%d" % i, [64, 512], F32) for i in range(2)]
    aT = [A.sb("gl_aT%d" % i, [16, 512], F32) for i in range(2)]
    tri16 = A.sb("gl_tri16", [128, 128], F32)
    all16 = A.sb("gl_all16", [128, 128], F32)
    identb = A.sb("gl_identb", [128, 128], BF16)
    ones_row = A.sb("gl_ones", [1, 128], F32)
    Sst = A.sb("gl_S", [64, 128], F32)
    Sbf = A.sb("gl_Sbf", [64, 128], BF16)
    stage = [A.sb("gl_stage%d" % i, [128, 512], BF16) for i in range(2)]
    R = 3
    k_sb = [A.sb("gl_k%d" % i, [128, 64], F32) for i in range(R)]
    v_bf = [A.sb("gl_v%d" % i, [128, 128], BF16) for i in range(R)]
    sog = [A.sb("gl_sog%d" % i, [128, 128], F32) for i in range(R)]
    gs = [A.sb("gl_gs%d" % i, [128, 128], F32) for i in range(R)]
    ee = [A.sb("gl_e%d" % i, [128, 64], F32) for i in range(R)]
    LL = [A.sb("gl_L%d" % i, [128, 64], F32) for i in range(R)]
    EcT = [A.sb("gl_EcT%d" % i, [64, 128], F32) for i in range(R)]
    EncT = [A.sb("gl_EncT%d" % i, [64, 128], F32) for i in range(R)]
    qeT = [A.sb("gl_qeT%d" % i, [64, 128], BF16) for i in range(R)]
    keT = [A.sb("gl_keT%d" % i, [64, 128], BF16) for i in range(R)]
    cum_sb = [A.sb("gl_cum%d" % i, [128, 64], F32) for i in range(R)]
    dcum = [A.sb("gl_dcum%d" % i, [128, 64], F32) for i in range(R)]
    Ed = [A.sb("gl_Ed%d" % i, [128, 64], F32) for i in range(R)]
    kd = [A.sb("gl_kd%d" % i, [128, 64], BF16) for i in range(R)]
    attT = [A.sb("gl_attT%d" % i, [128, 128], BF16) for i in range(R)]
    junk = [A.sb("gl_junk%d" % i, [128, 128], F32) for i in range(R)]
    ss = [A.sb("gl_ss%d" % i, [128, 1], F32) for i in range(R)]
    rstd = [A.sb("gl_rstd%d" % i, [128, 1], F32) for i in range(R)]
    oa = [A.sb("gl_oa%d" % i, [128, 128], BF16) for i in range(R)]
    psr = Rot([A.ps("gl_ps%d" % i, [128, 512], F32) for i in range(8)])

    P.ts(tri16[:], cst[:, 0, :], -1.0 / 16.0, None, ALU.mult, eng="dve")
    P.ts(all16[:], cst[:, 2, :], -1.0 / 16.0, None, ALU.mult, eng="dve")
    P.copy(identb[:], cst[:, 3, :], eng="dve")
    P.memset(ones_row[:], 1.0, eng="pool")
    P.memset(Sst[:], 0.0, eng="pool")
    P.memset(Sbf[:], 0.0, eng="pool")
    uT_v = uT_d.rearrange("(k p) t -> p k t", p=128)
    w_alpha = wsm[0:16, 0, 0:64]
    b_alpha = prow[0:1, PROW["b_alpha"][0]:PROW["b_alpha"][1]]
    norm_g = pbc[:, PBC["norm_g"][0]:PBC["norm_g"][1]]
    ti = 0
    import os as _os
    _gd = int(_os.environ.get("GLA_DBG", "99"))
    for tb in range(NB):
        u = ub[tb % 2]
        P.dma(u[:], uT_v[:, :, tb * 512:(tb + 1) * 512], key="gl_u%d" % (tb % 2), q="sp")
        for nm, dst, m in (("gq", qT[tb % 2], 64), ("gk", kT[tb % 2], 64), ("ga", aT[tb % 2], 16)):
            c0, c1 = FM_COLS[nm]
            pp = psr.next()
            for k in range(8):
                P.mm(pp[0:m, :], wfm_sb[:, k, c0:c1], u[:, k, :], start=(k == 0), stop=(k == 7))
            P.copy(dst[:, :], pp[0:m, :], eng="act")
        st = stage[tb % 2]
        for t4 in range(4):
            i = ti % R
            ti += 1
            ts_ = slice(t4 * 128, (t4 + 1) * 128)
            if _gd <= 0:
                continue
            p1 = psr.next()
            for k in range(8):
                P.mm(p1[:, 0:320], u[:, k, ts_], wtm_sb[:, k, 0:320], start=(k == 0), stop=(k == 7))
            _gs = _os.environ.get("GLA_SKIP", "")
            if "a" not in _gs:
                P.copy(k_sb[i][:], p1[:, 0:64], eng="act")
            if "b" not in _gs:
                P.copy(v_bf[i][:], p1[:, 64:192], eng="dve")
            if "c" not in _gs:
                P.act(sog[i][:], p1[:, 192:320], AF.Silu)
            if "d" not in _gs:
                P.tt(gs[i][:], sog[i][:], norm_g, ALU.mult, eng="pool")
            if _gd <= 1:
                continue
            p2 = psr.next()
            P.mm(p2[:, 0:64], aT[tb % 2][:, ts_], w_alpha, start=True, stop=False)
            P.mm(p2[:, 0:64], ones_row[:], b_alpha, start=False, stop=True)
            P.act(ee[i][:], p2[:, 0:64], AF.Exp, scale=-1.0)
            P.act(LL[i][:], ee[i][:], AF.Ln, bias=1.0)
            if _gd <= 2:
                continue
            p3 = psr.next()
            P.mm(p3[0:64, 0:128], LL[i][:], tri16[:])
            p4 = psr.next()
            P.mm(p4[:, 0:64], tri16[:], LL[i][:])
            P.mm(p4[:, 64:128], all16[:], LL[i][:])
            P.act(EcT[i][:], p3[0:64, 0:128], AF.Exp)
            P.act(EncT[i][:], p3[0:64, 0:128], AF.Exp, scale=-1.0)
            P.stt(qeT[i][:], qT[tb % 2][:, ts_], 0.125, EcT[i][:], ALU.mult, ALU.mult, eng="dve")
            P.tt(keT[i][:], kT[tb % 2][:, ts_], EncT[i][:], ALU.mult, eng="pool")
            P.copy(cum_sb[i][:], p4[:, 0:64], eng="act")
            P.tt(dcum[i][:], p4[:, 64:128], cum_sb[i][:], ALU.subtract, eng="dve")
            P.act(Ed[i][:], dcum[i][:], AF.Exp)
            P.tt(kd[i][:], k_sb[i][:], Ed[i][:], ALU.mult, eng="pool")
            if _gd <= 3:
                continue
            p5 = psr.next()
            P.mm(p5[:, 0:128], keT[i][:], qeT[i][:])
            P.tt(attT[i][:], p5[:, 0:128], cst[:, 5, :], ALU.mult, eng="dve")
            if _gd <= 4:
                continue
            p6 = psr.next()
            P.mm(p6[:, 0:128], attT[i][:], v_bf[i][:], start=True, stop=False)
            P.mm(p6[:, 0:128], qeT[i][:], Sbf[:], start=False, stop=True)
            if _gd <= 5:
                continue
            p7 = psr.next()
            P.mm(p7[0:64, 0:128], kd[i][:], v_bf[i][:])
            P.stt(Sst[:], Sst[:], EcT[i][:, 127:128], p7[0:64, 0:128], ALU.mult, ALU.add, eng="dve")
            P.copy(Sbf[:], Sst[:], eng="act")
            if _gd <= 6:
                continue
            P.act(junk[i][:], p6[:, 0:128], AF.Square, accum_out=ss[i][:])
            P.act(rstd[i][:], ss[i][:], AF.Ln, scale=1.0 / 128.0, bias=1e-5)
            P.act(rstd[i][:], rstd[i][:], AF.Exp, scale=-0.5)
            P.stt(oa[i][:], p6[:, 0:128], rstd[i][:], gs[i][:], ALU.mult, ALU.mult, eng="dve")
            if _gd <= 7:
                continue
            p8 = psr.next()
            P.mm(p8[:, 0:128], oa[i][:], identb[:])
            P.copy(st[:, ts_], p8[:, 0:128], eng="act")
        P.dma(outT_d[0:128, tb * 512:(tb + 1) * 512], st[:], key="gl_o%d" % (tb % 2), q="sp")
    P.barrier()
    A.close()


def bc_last(ap, n):
    dims = [list(x) for x in ap.ap]
    return bass.AP(ap.tensor, ap.offset, dims + [[0, n]])


def emit_rwkv(nc, P, S, uT_d, wfm_sb, wtm_sb, cst, prow, pbc, ppp, wsm, outT_d):
    A = Alloc(nc)
    NB = S // 512
    sb = A.sb
    ub = [sb("rw_u%d" % i, [128, 8, 513], BF16) for i in range(2)]
    twT = [sb("rw_twT%d" % i, [64, 512], F32) for i in range(2)]
    alT = [sb("rw_alT%d" % i, [64, 512], F32) for i in range(2)]
    sgT = [sb("rw_sgT%d" % i, [128, 512], F32) for i in range(2)]
    lA = sb("rw_lA", [128, 512], F32)
    lD = sb("rw_lD", [128, 512], F32)
    triw = sb("rw_triw", [128, 3, 128], F32)
    allw = sb("rw_allw", [128, 1], F32)
    nm1 = sb("rw_nm1", [128, 2, 128], F32)
    nmls = sb("rw_nmls", [128, 128], F32)
    identb = sb("rw_identb", [128, 128], BF16)
    ones_row = sb("rw_ones", [1, 128], F32)
    omka = sb("rw_omka", [128, 128], F32)
    Tst = [[sb("rw_T%d_%d" % (h, i), [64, 64], F32) for i in range(2)] for h in range(2)]
    stage = [sb("rw_stage%d" % i, [128, 512], BF16) for i in range(2)]
    R = 2
    def mk(nm, shape, dt=F32):
        return [sb("rw_%s%d" % (nm, i), shape, dt) for i in range(R)]
    P_sb = mk("P", [128, 384]); dd = mk("dd", [128, 384]); rkv = mk("rkv", [128, 384])
    sa = mk("sa", [128, 2, 128]); g_sb = mk("g", [128, 128])
    Wall = mk("Wall", [128, 3, 128]); Winv = mk("Winv", [128, 128]); WCc = mk("WCc", [64, 2])
    kk = mk("kk", [128, 128]); kk2 = mk("kk2", [128, 128]); ssq = mk("ssq", [128, 2]); rn = mk("rn", [128, 2])
    kkn = mk("kkn", [128, 128]); bb = mk("bb", [128, 128]); t1 = mk("t1", [128, 128]); kt = mk("kt", [128, 128])
    At = mk("At", [128, 128]); Bt = mk("Bt", [128, 128]); Kh = mk("Kh", [128, 128]); Rt = mk("Rt", [128, 128])
    Kb = mk("Kb", [128, 128]); Bbn = mk("Bbn", [128, 128])
    ATs = [mk("ATs%d" % h, [64, 4, 128]) for h in range(2)]
    ZN = [mk("ZN%d" % h, [128, 2, 128]) for h in range(2)]
    KR = [mk("KR%d" % h, [128, 2, 128]) for h in range(2)]
    Xp = [[mk("Xp%d_%d" % (h, j), [128, 128]) for j in range(2)] for h in range(2)]
    Zp = [[mk("Zp%d_%d" % (h, j), [128, 128]) for j in range(2)] for h in range(2)]
    Rb = [[mk("Rb%d_%d" % (h, j), [128, 128]) for j in range(2)] for h in range(2)]
    GT = [mk("GT%d" % h, [64, 64]) for h in range(2)]
    QhT = [mk("QhT%d" % h, [64, 128]) for h in range(2)]
    Ysb = mk("Y", [128, 128]); ysq = mk("ysq", [128, 128]); s1 = mk("s1", [128, 2]); s2 = mk("s2", [128, 2])
    mean = mk("mean", [128, 2]); msq = mk("msq", [128, 2]); var = mk("var", [128, 2]); rstd = mk("rstd", [128, 2])
    yn = mk("yn", [128, 128]); rk = mk("rk", [128, 128]); rks = mk("rks", [128, 2]); bon = mk("bon", [128, 128])
    ob = mk("ob", [128, 128], BF16)
    psr = Rot([A.ps("rw_ps%d" % i, [128, 512], F32) for i in range(8)])

    def pbcs(nm):
        return pbc[:, PBC[nm][0]:PBC[nm][1]]

    P.ts(triw[:, 0, :], cst[:, 0, :], WDEC, None, ALU.mult, eng="dve")
    P.ts(triw[:, 1, :], cst[:, 1, :], WDEC, None, ALU.mult, eng="dve")
    P.ts(triw[:, 2, :], cst[:, 6, :], WDEC, None, ALU.mult, eng="dve")
    P.memset(allw[:], WDEC, eng="pool")
    P.ts(nm1[:, 0, :], cst[:, 4, :], -1.0, None, ALU.mult, eng="dve")
    P.ts(nm1[:, 1, :], cst[:, 5, :], -1.0, None, ALU.mult, eng="dve")
    P.ts(nmls[:], cst[:, 6, :], -1.0, None, ALU.mult, eng="dve")
    P.copy(identb[:], cst[:, 3, :], eng="dve")
    P.memset(ones_row[:], 1.0, eng="pool")
    P.ts(omka[:], pbcs("k_a"), -1.0, 1.0, ALU.mult, ALU.add, eng="dve")
    for h in range(2):
        P.memset(Tst[h][0][:], 0.0, eng="pool")
    ident = cst[:, 3, :]
    m2 = cst[:, 4:6, :]
    uT_v = uT_d.rearrange("(k p) t -> p k t", p=128)
    w_up = wsm[0:64, 1, :]
    a_up = wsm[0:64, 2, :]
    g_up = wsm[:, 3, :]
    w0_row = prow[0:1, PROW["w0"][0]:PROW["w0"][1]]
    a0_row = prow[0:1, PROW["a0"][0]:PROW["a0"][1]]
    ev = Rot(["act", "dve"])
    ci = 0
    for tb in range(NB):
        u = ub[tb % 2]
        t0 = tb * 512
        if tb == 0:
            P.memset(u[:, :, 0:1], 0.0, eng="pool")
            P.dma(u[:, :, 1:513], uT_v[:, :, 0:512], key="rw_u%d" % (tb % 2), q="sp")
        else:
            P.dma(u[:, :, :], uT_v[:, :, t0 - 1:t0 + 512], key="rw_u%d" % (tb % 2), q="sp")
        for nm, c0, m, pc, dst, fn in (("w", 144, 64, 0, twT[tb % 2], AF.Tanh),
                                       ("a", 208, 64, 1, alT[tb % 2], None),
                                       ("g", 272, 128, 2, sgT[tb % 2], AF.Sigmoid)):
            pa = psr.next()
            for k in range(8):
                P.mm(pa[0:m, :], wfm_sb[:, k, c0:c0 + m], u[:, k, 1:513], start=(k == 0), stop=(k == 7))
            pb_ = psr.next()
            for k in range(8):
                P.mm(pb_[0:m, :], wfm_sb[:, k, c0:c0 + m], u[:, k, 0:512], start=(k == 0), stop=(k == 7))
            P.copy(lA[0:m, :], pa[0:m, :], eng="act")
            P.tt(lD[0:m, :], pb_[0:m, :], lA[0:m, :], ALU.subtract, eng="dve")
            if fn is None:
                P.stt(dst[:, :], lD[0:m, :], ppp[0:m, pc:pc + 1], lA[0:m, :], ALU.mult, ALU.add, eng="dve")
            else:
                P.stt(lD[0:m, :], lD[0:m, :], ppp[0:m, pc:pc + 1], lA[0:m, :], ALU.mult, ALU.add, eng="dve")
                P.act(dst[:, :], lD[0:m, :], fn)
        st = stage[tb % 2]
        for t4 in range(4):
            i = ci % R
            ci += 1
            ts_ = slice(t4 * 128, (t4 + 1) * 128)
            tsc = slice(1 + t4 * 128, 1 + (t4 + 1) * 128)
            tsp = slice(t4 * 128, (t4 + 1) * 128)
            pa = psr.next()
            for k in range(8):
                P.mm(pa[:, 0:384], u[:, k, tsc], wtm_sb[:, k, 320:704], start=(k == 0), stop=(k == 7))
            pb_ = psr.next()
            for k in range(8):
                P.mm(pb_[:, 0:384], u[:, k, tsp], wtm_sb[:, k, 320:704], start=(k == 0), stop=(k == 7))
            P.copy(P_sb[i][:], pa[:, 0:384], eng="act")
            P.tt(dd[i][:], pb_[:, 0:384], P_sb[i][:], ALU.subtract, eng="dve")
            P.tt(dd[i][:], dd[i][:], pbcs("mu_rkv"), ALU.mult, eng="pool")
            P.tt(rkv[i][:], P_sb[i][:], dd[i][:], ALU.add, eng="pool")
            r_ = rkv[i][:, 0:128]
            k_ = rkv[i][:, 128:256]
            v_ = rkv[i][:, 256:384]
            pz = psr.next()
            P.mm(pz[:, 0:128], twT[tb % 2][:, ts_], w_up, start=True, stop=False)
            P.mm(pz[:, 0:128], ones_row[:], w0_row, start=False, stop=True)
            P.mm(pz[:, 128:256], alT[tb % 2][:, ts_], a_up, start=True, stop=False)
            P.mm(pz[:, 128:256], ones_row[:], a0_row, start=False, stop=True)
            P.mm(pz[:, 256:384], sgT[tb % 2][:, ts_], g_up, start=True, stop=True)
            P.act(sa[i][:].rearrange("p a b -> p (a b)"), pz[:, 0:256], AF.Sigmoid)
            P.copy(g_sb[i][:], pz[:, 256:384], eng="dve")
            sig = sa[i][:, 0, :]
            a_ = sa[i][:, 1, :]
            pc_ = psr.next()
            for j in range(3):
                P.mm(pc_[:, j * 128:(j + 1) * 128], triw[:, j, :], sig)
            P.act(Wall[i][:].rearrange("p a b -> p (a b)"), pc_[:, 0:384], AF.Exp)
            P.act(Winv[i][:], pc_[:, 0:128], AF.Exp, scale=-1.0)
            pw = psr.next()
            for h in range(2):
                P.mm(pw[0:64, h:h + 1], sa[i][:, 0, 64 * h:64 * h + 64], allw[:])
            P.act(WCc[i][:], pw[0:64, 0:2], AF.Exp)
            W_ = Wall[i][:, 0, :]
            Wex = Wall[i][:, 1, :]
            WCr = Wall[i][:, 2, :]
            P.tt(kk[i][:], k_, pbcs("k_k"), ALU.mult, eng="dve")
            P.tt(kk2[i][:], kk[i][:], kk[i][:], ALU.mult, eng="pool")
            P.reduce(ssq[i][:], kk2[i][:].rearrange("p (h c) -> p h c", h=2), ALU.add, eng="dve")
            P.act(rn[i][:], ssq[i][:], AF.Ln, bias=1e-24)
            P.act(rn[i][:], rn[i][:], AF.Exp, scale=-0.5)
            P.tt(kkn[i][:].rearrange("p (h c) -> p h c", h=2), kk[i][:].rearrange("p (h c) -> p h c", h=2),
                 bc_last(rn[i][:], 64), ALU.mult, eng="dve")
            P.tt(bb[i][:], kkn[i][:], a_, ALU.mult, eng="pool")
            P.tt(t1[i][:], a_, pbcs("k_a"), ALU.mult, eng="dve")
            P.tt(t1[i][:], t1[i][:], omka[:], ALU.add, eng="pool")
            P.tt(kt[i][:], k_, t1[i][:], ALU.mult, eng="pool")
            P.tt(At[i][:], kkn[i][:], Wex, ALU.mult, eng="dve")
            P.tt(Bt[i][:], bb[i][:], Winv[i][:], ALU.mult, eng="pool")
            P.tt(Kh[i][:], kt[i][:], Winv[i][:], ALU.mult, eng="dve")
            P.tt(Rt[i][:], r_, W_, ALU.mult, eng="pool")
            P.tt(Kb[i][:], kt[i][:], WCr, ALU.mult, eng="dve")
            P.stt(Bbn[i][:], bb[i][:], -1.0, WCr, ALU.mult, ALU.mult, eng="dve")
            for h in range(2):
                hc = slice(64 * h, 64 * h + 64)
                pt = psr.next()
                for q, X in enumerate((At, Rt, Bt, Kh)):
                    P.mm(pt[0:64, q * 128:(q + 1) * 128], X[i][:, hc], ident)
                P.copy(ATs[h][i][:].rearrange("p a b -> p (a b)"), pt[0:64, :], eng=ev.next())
            for h in range(2):
                hc = slice(64 * h, 64 * h + 64)
                AR = ATs[h][i][:, 0:2, :].rearrange("p a b -> p (a b)")
                AtT = ATs[h][i][:, 0, :]
                BtT = ATs[h][i][:, 2, :]
                KhT = ATs[h][i][:, 3, :]
                p1 = psr.next()
                P.mm(p1[:, 0:256], BtT, AR)
                P.tt(ZN[h][i][:].rearrange("p a b -> p (a b)"), p1[:, 0:256], nm1[:].rearrange("p a b -> p (a b)"),
                     ALU.mult, eng="dve")
                p2 = psr.next()
                P.mm(p2[:, 0:256], KhT, AR)
                P.tt(KR[h][i][:].rearrange("p a b -> p (a b)"), p2[:, 0:256], m2.rearrange("p a b -> p (a b)"),
                     ALU.mult, eng="dve")
                p3 = psr.next()
                P.mm(p3[:, 0:128], AtT, BtT)
                P.tt(Xp[h][0][i][:], p3[:, 0:128], nmls[:], ALU.mult, eng="dve")
                p4 = psr.next()
                P.mm(p4[:, 0:64], KR[h][i][:, 0, :], v_[:, hc])
                P.copy(Rb[h][0][i][:, 64:128], p4[:, 0:64], eng="act")
                P.copy(Rb[h][0][i][:, 0:64], At[i][:, hc], eng="pool")
            zc = [ZN[h][i][:, 0, :] for h in range(2)]
            xc = [Xp[h][0][i][:] for h in range(2)]
            rc = [0, 0]
            for j in range(7):
                for h in range(2):
                    pj = psr.next()
                    P.mm(pj[:, 0:128], zc[h], Rb[h][rc[h]][i][:])
                    P.tt(Rb[h][1 - rc[h]][i][:], Rb[h][rc[h]][i][:], pj[:, 0:128], ALU.add, eng="dve")
                    rc[h] = 1 - rc[h]
                    if j < 6:
                        pq = psr.next()
                        P.mm(pq[:, 0:128], xc[h], zc[h])
                        P.mm(pq[:, 128:256], zc[h], xc[h])
                        zn_ = Zp[h][j % 2][i][:]
                        xn_ = Xp[h][(j + 1) % 2][i][:]
                        P.copy(zn_, pq[:, 0:128], eng="act")
                        P.copy(xn_, pq[:, 128:256], eng="act")
                        zc[h] = zn_
                        xc[h] = xn_
            py = psr.next()
            cur = (ci - 1) % 2
            for h in range(2):
                hc = slice(64 * h, 64 * h + 64)
                Rf = Rb[h][rc[h]][i]
                Ah = Rf[:, 0:64]
                P0 = Rf[:, 64:128]
                nArbT = ZN[h][i][:, 1, :]
                ArT = KR[h][i][:, 1, :]
                Tcur = Tst[h][cur]
                Tnxt = Tst[h][1 - cur]
                pg = psr.next()
                P.mm(pg[0:64, 0:64], Ah, Bbn[i][:, hc])
                P.stt(GT[h][i][:], ident[0:64, 0:64], WCc[i][:, h:h + 1], pg[0:64, 0:64], ALU.mult, ALU.add, eng="dve")
                pq_ = psr.next()
                P.mm(pq_[0:64, 0:128], Rt[i][:, hc], ident, start=True, stop=False)
                P.mm(pq_[0:64, 0:128], Ah, nArbT, start=False, stop=True)
                P.copy(QhT[h][i][:], pq_[0:64, 0:128], eng="act")
                P.mm(py[:, hc], QhT[h][i][:], Tcur[:], start=True, stop=False)
                P.mm(py[:, hc], ArT, v_[:, hc], start=False, stop=False)
                P.mm(py[:, hc], nArbT, P0, start=False, stop=True)
                pt_ = psr.next()
                P.mm(pt_[0:64, 0:64], GT[h][i][:], Tcur[:], start=True, stop=False)
                P.mm(pt_[0:64, 0:64], Kb[i][:, hc], v_[:, hc], start=False, stop=False)
                P.mm(pt_[0:64, 0:64], Bbn[i][:, hc], P0, start=False, stop=True)
                P.copy(Tnxt[:], pt_[0:64, 0:64], eng="dve")
            Y3 = Ysb[i][:].rearrange("p (h c) -> p h c", h=2)
            P.copy(Ysb[i][:], py[:, 0:128], eng="act")
            P.reduce(s1[i][:], Y3, ALU.add, eng="dve")
            P.tt(ysq[i][:], Ysb[i][:], Ysb[i][:], ALU.mult, eng="pool")
            P.reduce(s2[i][:], ysq[i][:].rearrange("p (h c) -> p h c", h=2), ALU.add, eng="dve")
            P.ts(mean[i][:], s1[i][:], 1.0 / 64.0, None, ALU.mult, eng="dve")
            P.tt(msq[i][:], mean[i][:], mean[i][:], ALU.mult, eng="dve")
            P.stt(var[i][:], s2[i][:], 1.0 / 64.0, msq[i][:], ALU.mult, ALU.subtract, eng="dve")
            P.act(rstd[i][:], var[i][:], AF.Ln, bias=64e-5)
            P.act(rstd[i][:], rstd[i][:], AF.Exp, scale=-0.5)
            yn3 = yn[i][:].rearrange("p (h c) -> p h c", h=2)
            P.tt(yn3, Y3, bc_last(mean[i][:], 64), ALU.subtract, eng="dve")
            P.tt(yn3, yn3, bc_last(rstd[i][:], 64), ALU.mult, eng="dve")
            P.tt(yn[i][:], yn[i][:], pbcs("gn_g"), ALU.mult, eng="pool")
            P.tt(yn[i][:], yn[i][:], pbcs("gn_b"), ALU.add, eng="pool")
            P.tt(rk[i][:], r_, kt[i][:], ALU.mult, eng="pool")
            P.tt(rk[i][:], rk[i][:], pbcs("r_k"), ALU.mult, eng="pool")
            P.reduce(rks[i][:], rk[i][:].rearrange("p (h c) -> p h c", h=2), ALU.add, eng="dve")
            P.tt(bon[i][:].rearrange("p (h c) -> p h c", h=2), v_.rearrange("p (h c) -> p h c", h=2),
                 bc_last(rks[i][:], 64), ALU.mult, eng="dve")
            P.tt(yn[i][:], yn[i][:], bon[i][:], ALU.add, eng="pool")
            P.tt(ob[i][:], yn[i][:], g_sb[i][:], ALU.mult, eng="dve")
            po = psr.next()
            P.mm(po[:, 0:128], ob[i][:], identb[:])
            P.copy(st[:, ts_], po[:, 0:128], eng="act")
        P.dma(outT_d[128:256, tb * 512:(tb + 1) * 512], st[:], key="rw_o%d" % (tb % 2), q="sp")
    P.barrier()
    A.close()


FFN_PIECES = [(0, 4), (4, 8), (8, 12), (12, 16), (16, 20), (20, 22)]


class DenseBufs:
    def __init__(self, nc, A, NT):
        sb = A.sb
        self.NT = NT
        ntl = NT // 128
        self.T1 = sb("dn_T1", [128, 8, NT], BF16)
        self.T2 = sb("dn_T2", [128, max(12 * NT, 8192)], BF16)
        self.T3 = sb("dn_T3", [128, 8, NT], BF16)
        self.X = sb("dn_X", [128, ntl, 1024], F32)
        self.wg = sb("dn_wg", [128, 3, 8, 128], BF16)
        self.wb = sb("dn_wb", [128, 3, 4, 128], BF16)
        self.w1p = [sb("dn_w1p%d" % i, [128, 8, 8, 128], BF16) for i in range(2)]
        self.w2p = [sb("dn_w2p%d" % i, [128, 4, 1024], BF16) for i in range(2)]
        self.bc = sb("dn_bc", [128, 3, 1024], F32)
        self.sg = [sb("dn_sg%d" % i, [128, 512], F32) for i in range(2)]
        self.tmp = [sb("dn_tmp%d" % i, [128, 512], F32) for i in range(2)]
        self.actT = [sb("dn_actT%d" % i, [128, 4, 512], BF16) for i in range(2)]
        self.junk = sb("dn_junk", [128, 1024], F32)
        self.st = [sb("dn_st%d" % i, [128, 8], F32) for i in range(2)]
        self.modp = sb("dn_modp", [128, 4, 8], F32)
        self.ps = [A.ps("dn_ps%d" % i, [128, 512], F32) for i in range(8)]


def _layernorm_tile(P, B, xt, gb, bb_, si):
    st = B.st[si % 2]
    P.reduce(st[:, 0:1], xt, ALU.add, eng="dve")
    P.act(B.junk[:], xt, AF.Square, accum_out=st[:, 1:2])
    P.ts(st[:, 2:3], st[:, 0:1], 1.0 / 1024.0, None, ALU.mult, eng="dve")
    P.tt(st[:, 3:4], st[:, 2:3], st[:, 2:3], ALU.mult, eng="dve")
    P.stt(st[:, 4:5], st[:, 1:2], 1.0 / 1024.0, st[:, 3:4], ALU.mult, ALU.subtract, eng="dve")
    P.act(st[:, 5:6], st[:, 4:5], AF.Ln, bias=1e-5)
    P.act(st[:, 5:6], st[:, 5:6], AF.Exp, scale=-0.5)
    P.ts(xt, xt, st[:, 2:3], st[:, 5:6], ALU.subtract, ALU.mult, eng="dve")
    P.tt(xt, xt, gb, ALU.mult, eng="pool")
    P.tt(xt, xt, bb_, ALU.add, eng="pool")


def _transpose_mod(P, B, xt, dstT, tcols, scp, shp, ident, psr):
    for half in range(2):
        pb = psr.next()
        for q in range(4):
            fc = half * 4 + q
            P.mm(pb[:, q * 128:(q + 1) * 128], xt[:, fc * 128:(fc + 1) * 128], ident)
        for q in range(4):
            fc = half * 4 + q
            P.ts(dstT[:, fc, tcols], pb[:, q * 128:(q + 1) * 128], scp[:, fc:fc + 1], shp[:, fc:fc + 1],
                 ALU.mult, ALU.add, eng="dve")


def emit_dense(nc, P, B, cst, x_d, uT_d, brT_d, w_in_d, w_branch_d, w_out_d, w1_d, w2_d,
               bcv_d, modp_d, xo_d, uTn_d, tag):
    NT = B.NT
    ntl = NT // 128
    ntg = NT // 512
    psr = Rot(B.ps)
    ident = cst[:, 3, :]
    k = lambda s: "dn_%s" % s
    P.dma(B.T1[:], uT_d.rearrange("(k p) t -> p k t", p=128), key=k("T1"), q="sp")
    brv = B.T2[:, 0:12 * NT].rearrange("p (c t) -> p c t", c=12)
    P.dma(brv, brT_d.rearrange("(c p) t -> p c t", p=128), key=k("T2"), q="sp")
    P.dma(B.X[:], x_d.rearrange("(t p) f -> p t f", p=128), key=k("X"), q="sp")
    P.dma(B.bc[:], bcv_d[0:3].rearrange("a p f -> p a f"), key=k("bc"), q="sp")
    P.dma(B.modp[:].rearrange("p a b -> p (a b)"), modp_d, key=k("modp"), q="sp")
    P.ts(B.modp[:, 0, :], B.modp[:, 0, :], 1.0, None, ALU.add, eng="dve")
    P.ts(B.modp[:, 2, :], B.modp[:, 2, :], 1.0, None, ALU.add, eng="dve")
    P.ts(B.bc[:, 0, :], B.bc[:, 0, :], 1.0, None, ALU.add, eng="dve")
    for fc in range(8):
        for n in range(3):
            c0 = n * 1024 + fc * 128
            P.dma(B.wg[:, n], w_in_d[:, c0:c0 + 128].rearrange("(k p) c -> p k c", p=128), key=k("wg%d" % n), q="pool")
            P.dma(B.wb[:, n], w_branch_d[n, :, fc * 128:(fc + 1) * 128].rearrange("(k p) c -> p k c", p=128),
                  key=k("wb%d" % n), q="pool")
        for tg in range(ntg):
            tc_ = slice(tg * 512, (tg + 1) * 512)
            for n in range(3):
                pg = psr.next()
                for kk_ in range(8):
                    P.mm(pg[:, :], B.wg[:, n, kk_, :], B.T1[:, kk_, tc_], start=(kk_ == 0), stop=(kk_ == 7))
                sg = B.sg[n % 2]
                P.act(sg[:], pg[:, :], AF.Sigmoid)
                pp = psr.next()
                for kk_ in range(4):
                    P.mm(pp[:, :], B.wb[:, n, kk_, :], brv[:, n * 4 + kk_, tc_], start=(kk_ == 0), stop=(kk_ == 3))
                if n == 0:
                    P.tt(B.tmp[0][:], sg[:], pp[:, :], ALU.mult, eng="dve")
                elif n == 1:
                    P.tt(B.tmp[1][:], sg[:], pp[:, :], ALU.mult, eng="dve")
                    P.tt(B.tmp[0][:], B.tmp[0][:], B.tmp[1][:], ALU.add, eng="pool")
                else:
                    P.tt(B.tmp[1][:], sg[:], pp[:, :], ALU.mult, eng="dve")
                    P.tt(B.T3[:, fc, tc_], B.tmp[0][:], B.tmp[1][:], ALU.add, eng="pool")
    wo = B.T2[:, 0:8 * 1024].rearrange("p (k c) -> p k c", k=8)
    P.dma(wo, w_out_d.rearrange("(k p) c -> p k c", p=128), key=k("T2"), q="pool")
    for t in range(ntl):
        tcols = slice(t * 128, (t + 1) * 128)
        xt = B.X[:, t, :]
        for cg in range(2):
            cc = slice(cg * 512, (cg + 1) * 512)
            ph = psr.next()
            for fc in range(8):
                P.mm(ph[:, :], B.T3[:, fc, tcols], wo[:, fc, cc], start=(fc == 0), stop=(fc == 7))
            P.tt(B.tmp[cg][:], ph[:, :], B.bc[:, 0, cc], ALU.mult, eng="dve")
            P.stt(B.X[:, t, cc], B.X[:, t, cc], DEEPNORM_ALPHA, B.tmp[cg][:], ALU.mult, ALU.add, eng="dve")
        _layernorm_tile(P, B, xt, B.bc[:, 1, :], B.bc[:, 2, :], t)
        _transpose_mod(P, B, xt, B.T1, tcols, B.modp[:, 0, :], B.modp[:, 1, :], ident, psr)
        P.act(xt, xt, AF.Copy, scale=DEEPNORM_ALPHA)
    P.dma(B.bc[:], bcv_d[3:6].rearrange("a p f -> p a f"), key=k("bc"), q="sp")
    P.ts(B.bc[:, 0, :], B.bc[:, 0, :], 1.0, None, ALU.add, eng="dve")
    for pi, (j0, j1) in enumerate(FFN_PIECES):
        nj = j1 - j0
        w1p = B.w1p[pi % 2]
        w2p = B.w2p[pi % 2]
        P.dma(w1p[:, :, 0:nj, :], w1_d[:, j0 * 128:j1 * 128].rearrange("(k p) (j c) -> p k j c", p=128, c=128),
              key=k("w1g%d" % (pi % 2)), q="pool")
        P.dma(w1p[:, :, 4:4 + nj, :],
              w1_d[:, FFN_HIDDEN + j0 * 128:FFN_HIDDEN + j1 * 128].rearrange("(k p) (j c) -> p k j c", p=128, c=128),
              key=k("w1u%d" % (pi % 2)), q="pool")
        P.dma(w2p[:, 0:nj, :], w2_d[j0 * 128:j1 * 128, :].rearrange("(j p) c -> p j c", p=128),
              key=k("w2%d" % (pi % 2)), q="pool")
        for tg in range(ntg):
            tc_ = slice(tg * 512, (tg + 1) * 512)
            aT = B.actT[tg % 2]
            for jj in range(nj):
                pg = psr.next()
                for kk_ in range(8):
                    P.mm(pg[:, :], w1p[:, kk_, jj, :], B.T1[:, kk_, tc_], start=(kk_ == 0), stop=(kk_ == 7))
                pu = psr.next()
                for kk_ in range(8):
                    P.mm(pu[:, :], w1p[:, kk_, 4 + jj, :], B.T1[:, kk_, tc_], start=(kk_ == 0), stop=(kk_ == 7))
                sg = B.sg[jj % 2]
                P.act(sg[:], pg[:, :], AF.Silu)
                P.tt(aT[:, jj, :], sg[:], pu[:, :], ALU.mult, eng="dve")
            for tt_ in range(4):
                t = tg * 4 + tt_
                for cg in range(2):
                    cc = slice(cg * 512, (cg + 1) * 512)
                    pa = psr.next()
                    for jj in range(nj):
                        P.mm(pa[:, :], aT[:, jj, tt_ * 128:(tt_ + 1) * 128], w2p[:, jj, cc],
                             start=(jj == 0), stop=(jj == nj - 1))
                    P.tt(B.tmp[cg][:], pa[:, :], B.bc[:, 0, cc], ALU.mult, eng="dve")
                    P.tt(B.X[:, t, cc], B.X[:, t, cc], B.tmp[cg][:], ALU.add, eng="pool")
    for t in range(ntl):
        tcols = slice(t * 128, (t + 1) * 128)
        xt = B.X[:, t, :]
        _layernorm_tile(P, B, xt, B.bc[:, 1, :], B.bc[:, 2, :], t)
        _transpose_mod(P, B, xt, B.T3, tcols, B.modp[:, 2, :], B.modp[:, 3, :], ident, psr)
    P.dma(xo_d.rearrange("(t p) f -> p t f", p=128), B.X[:], key=k("xo"), q="sp")
    P.dma(uTn_d.rearrange("(k p) t -> p k t", p=128), B.T3[:], key=k("uo"), q="sp")


def emit_prologue(nc, P, B, cst, x_d, modp_d, uTn_d):
    NT = B.NT
    ntl = NT // 128
    psr = Rot(B.ps)
    ident = cst[:, 3, :]
    P.dma(B.X[:], x_d.rearrange("(t p) f -> p t f", p=128), key="pr_X", q="sp")
    P.dma(B.modp[:].rearrange("p a b -> p (a b)"), modp_d, key="pr_modp", q="sp")
    P.ts(B.modp[:, 2, :], B.modp[:, 2, :], 1.0, None, ALU.add, eng="dve")
    for t in range(ntl):
        tcols = slice(t * 128, (t + 1) * 128)
        _transpose_mod(P, B, B.X[:, t, :], B.T3, tcols, B.modp[:, 2, :], B.modp[:, 3, :], ident, psr)
    P.dma(uTn_d.rearrange("(k p) t -> p k t", p=128), B.T3[:], key="pr_uo", q="sp")


def _dram(nc, name, shape, dt, out=False):
    return nc.dram_tensor(name, list(shape), dt, kind="ExternalOutput" if out else "ExternalInput").ap()


def build_mod_program():
    nc = bass.Bass("TRN2", target_bir_lowering=False)
    cT_d = _dram(nc, "cT", [1024, 2], F32)
    w_d = _dram(nc, "w_ada", [DEPTH, 1024, 768], F32)
    b_d = _dram(nc, "b_ada", [1, DEPTH * 768], F32)
    o_d = _dram(nc, "mod", [2, DEPTH * 768], F32, out=True)
    P = Prog(nc)
    A = Alloc(nc)
    scT = A.sb("md_scT", [128, 8, 2], F32)
    ones2 = A.sb("md_ones", [1, 2], F32)
    brow = A.sb("md_brow", [1, DEPTH * 768], F32)
    w = [A.sb("md_w%d" % i, [128, 8, 768], F32) for i in range(2)]
    res = A.sb("md_res", [2, DEPTH * 768], F32)
    ps = [A.ps("md_ps%d" % i, [128, 512], F32) for i in range(4)]
    P.dma(scT[:], cT_d.rearrange("(k p) b -> p k b", p=128), key="md_c", q="sp")
    P.dma(brow[:], b_d, key="md_b", q="sp")
    P.memset(ones2[:], 1.0, eng="pool")
    P.act(scT[:].rearrange("p k b -> p (k b)"), scT[:].rearrange("p k b -> p (k b)"), AF.Silu)
    pi = 0
    for l in range(DEPTH):
        wl = w[l % 2]
        P.dma(wl[:], w_d[l].rearrange("(k p) n -> p k n", p=128), key="md_w%d" % (l % 2), q="sp")
        for (c0, c1) in ((0, 512), (512, 768)):
            pb = ps[pi % 4]
            pi += 1
            n = c1 - c0
            for k in range(8):
                P.mm(pb[0:2, 0:n], scT[:, k, :], wl[:, k, c0:c1], start=(k == 0), stop=False)
            P.mm(pb[0:2, 0:n], ones2[:], brow[0:1, l * 768 + c0:l * 768 + c1], start=False, stop=True)
            P.copy(res[:, l * 768 + c0:l * 768 + c1], pb[0:2, 0:n], eng="dve")
    P.dma(o_d, res[:], key="md_o", q="sp")
    P.finalize()
    A.close()
    return nc


def build_mixer_program(S=SEQ):
    nc = bass.Bass("TRN2", target_bir_lowering=False)
    uT_d = _dram(nc, "uT", [1024, S], BF16)
    wfm_d = _dram(nc, "wfm", [1024, NFM], F32)
    wtm_d = _dram(nc, "wtm", [1024, NTM], F32)
    cst_d = _dram(nc, "cst", [128, 8 * 128], F32)
    attb_d = _dram(nc, "attb", [128, 3 * 2 * 256], F32)
    prow_d = _dram(nc, "prow", [1, NROW], F32)
    pbc_d = _dram(nc, "pbc", [128, NBC], F32)
    ppp_d = _dram(nc, "ppp", [128, 3], F32)
    wsm_d = _dram(nc, "wsm", [128, 4 * 128], F32)
    outT_d = _dram(nc, "outT", [384, S], BF16, out=True)
    P = Prog(nc)
    A = Alloc(nc)
    wfm_sb = A.sb("wfm_sb", [128, 8, NFM], BF16)
    wtm_sb = A.sb("wtm_sb", [128, 8, NTM], BF16)
    cst = A.sb("cst_sb", [128, 8, 128], F32)
    prow_sb = A.sb("prow_sb", [1, NROW], F32)
    pbc_sb = A.sb("pbc_sb", [128, NBC], F32)
    wsm_sb = A.sb("wsm_sb", [128, 4, 128], F32)
    ppp_sb = A.sb("ppp_sb", [128, 3], F32)
    P.dma(wfm_sb[:], wfm_d.rearrange("(k p) n -> p k n", p=128), key="w", q="pool")
    P.dma(wtm_sb[:], wtm_d.rearrange("(k p) n -> p k n", p=128), key="w2", q="pool")
    P.dma(cst[:].rearrange("p a b -> p (a b)"), cst_d, key="c", q="sp")
    P.dma(prow_sb[:], prow_d, key="c1", q="sp")
    P.dma(pbc_sb[:], pbc_d, key="c2", q="sp")
    P.dma(wsm_sb[:].rearrange("p a b -> p (a b)"), wsm_d, key="c3", q="sp")
    P.dma(ppp_sb[:], ppp_d, key="c4", q="sp")
    emit_attention(nc, P, S, uT_d, wfm_sb, cst, attb_d, outT_d)
    emit_gla(nc, P, S, uT_d, wfm_sb, wtm_sb, cst, prow_sb, pbc_sb, wsm_sb, outT_d)
    emit_rwkv(nc, P, S, uT_d, wfm_sb, wtm_sb, cst, prow_sb, pbc_sb, ppp_sb, wsm_sb, outT_d)
    P.finalize()
    A.close()
    return nc


NTOK = SEQ * BATCH // NCORES
NT_HALF = 1024


def build_dense_program():
    nc = bass.Bass("TRN2", target_bir_lowering=False)
    x_d = _dram(nc, "x", [NTOK, 1024], F32)
    uT_d = _dram(nc, "uT", [1024, NTOK], BF16)
    brT_d = _dram(nc, "brT", [1536, NTOK], BF16)
    wg_d = _dram(nc, "w_gate", [1024, 3072], F32)
    wbr_d = _dram(nc, "w_branch", [3, 512, 1024], F32)
    wo_d = _dram(nc, "w_out", [1024, 1024], F32)
    w1_d = _dram(nc, "w1", [1024, 2 * FFN_HIDDEN], F32)
    w2_d = _dram(nc, "w2", [FFN_HIDDEN, 1024], F32)
    bcv_d = _dram(nc, "bcv", [6, 128, 1024], F32)
    modp_d = _dram(nc, "modp", [128, 32], F32)
    cst_d = _dram(nc, "cst", [128, 1024], F32)
    xo_d = _dram(nc, "xo", [NTOK, 1024], F32, out=True)
    uTn_d = _dram(nc, "uTn", [1024, NTOK], BF16, out=True)
    P = Prog(nc)
    A = Alloc(nc)
    cst = A.sb("cst_sb", [128, 8, 128], F32)
    P.dma(cst[:].rearrange("p a b -> p (a b)"), cst_d, key="c", q="sp")
    B = DenseBufs(nc, A, NT_HALF)
    for hf in range(NTOK // NT_HALF):
        ts_ = slice(hf * NT_HALF, (hf + 1) * NT_HALF)
        emit_dense(nc, P, B, cst, x_d[ts_, :], uT_d[:, ts_], brT_d[:, ts_], wg_d, wbr_d, wo_d, w1_d, w2_d,
                   bcv_d, modp_d, xo_d[ts_, :], uTn_d[:, ts_], "h%d" % hf)
    P.finalize()
    A.close()
    return nc


def build_prologue_program():
    nc = bass.Bass("TRN2", target_bir_lowering=False)
    x_d = _dram(nc, "x", [NTOK, 1024], F32)
    modp_d = _dram(nc, "modp", [128, 32], F32)
    cst_d = _dram(nc, "cst", [128, 1024], F32)
    uTn_d = _dram(nc, "uTn", [1024, NTOK], BF16, out=True)
    P = Prog(nc)
    A = Alloc(nc)
    cst = A.sb("cst_sb", [128, 8, 128], F32)
    P.dma(cst[:].rearrange("p a b -> p (a b)"), cst_d, key="c", q="sp")
    B = DenseBufs(nc, A, NT_HALF)
    for hf in range(NTOK // NT_HALF):
        ts_ = slice(hf * NT_HALF, (hf + 1) * NT_HALF)
        emit_prologue(nc, P, B, cst, x_d[ts_, :], modp_d, uTn_d[:, ts_])
    P.finalize()
    A.close()
    return nc


def _pp(v):
    return np.ascontiguousarray(v.reshape(8, 128).T)


def _bcrow(v):
    return np.ascontiguousarray(np.broadcast_to(v[None, :], (128, v.shape[0])))


def kernel(x, c, w_ada, b_ada, w_in, gla_w_alpha, gla_b_alpha, gla_norm_g, rwkv_mu, rwkv_w0,
           rwkv_w_up, rwkv_a0, rwkv_a_up, rwkv_g_up, rwkv_k_k, rwkv_k_a, rwkv_r_k, rwkv_gn_g,
           rwkv_gn_b, w_branch, w_out, ln1_g, ln1_b, ffn_w1, ffn_w2, ln2_g, ln2_b):
    f32 = np.float32
    A_ = lambda a: np.ascontiguousarray(np.asarray(a, dtype=f32))
    x = A_(x)
    cores = list(range(NCORES))
    cst = make_consts().reshape(128, -1)
    nc = build_mod_program()
    cT = np.ascontiguousarray(np.asarray(c, f32).T)
    w_ada = np.asarray(w_ada, f32)
    b_ada = np.asarray(b_ada, f32)
    maps = []
    for i in cores:
        cs = slice(768 * i, 768 * (i + 1))
        maps.append({"cT": cT, "w_ada": np.ascontiguousarray(w_ada[:, :, cs]),
                     "b_ada": np.ascontiguousarray(b_ada[:, cs]).reshape(1, -1)})
    res = run_bass_kernel_spmd(nc, maps, core_ids=cores).results
    mod = np.concatenate([r["mod"].reshape(2, DEPTH, 768) for r in res], axis=2)
    mv = lambda l, b, j: mod[b, l, j * 1024:(j + 1) * 1024]

    def modp_for(l, b):
        z = np.zeros(1024, f32)
        if l + 1 < DEPTH:
            scn, shn = mv(l + 1, b, 1), mv(l + 1, b, 0)
        else:
            scn, shn = z, z
        return np.ascontiguousarray(np.concatenate([_pp(mv(l, b, 4)), _pp(mv(l, b, 3)), _pp(scn), _pp(shn)], axis=1))

    nc = build_prologue_program()
    maps = []
    for i in cores:
        b, q = divmod(i, 4)
        z = np.zeros((128, 8), f32)
        mp = np.ascontiguousarray(np.concatenate([z, z, _pp(mv(0, b, 1)), _pp(mv(0, b, 0))], axis=1))
        maps.append({"x": x[b, q * NTOK:(q + 1) * NTOK], "modp": mp, "cst": cst})
    res = run_bass_kernel_spmd(nc, maps, core_ids=cores).results
    uT_sh = [r["uTn"] for r in res]
    x_sh = [x[i // 4, (i % 4) * NTOK:((i % 4) + 1) * NTOK] for i in cores]

    nc_mix = build_mixer_program()
    nc_dense = build_dense_program()
    w_in = np.asarray(w_in, f32)
    for l in range(DEPTH):
        uT_full = [np.ascontiguousarray(np.concatenate(uT_sh[4 * b:4 * b + 4], axis=1)) for b in range(BATCH)]
        maps = []
        for i in cores:
            b, g = divmod(i, 4)
            fm, tm = mixer_weight_cols(g)
            hs = slice(128 * g, 128 * g + 128)
            mu = np.asarray(rwkv_mu[l], f32)
            prow = np.zeros((1, NROW), f32)
            prow[0, 0:64] = np.asarray(gla_b_alpha[l], f32)[64 * g:64 * g + 64]
            prow[0, 64:192] = np.asarray(rwkv_w0[l], f32)[hs]
            prow[0, 192:320] = np.asarray(rwkv_a0[l], f32)[hs]
            pbc = np.zeros((128, NBC), f32)
            pbc[:, 0:128] = np.asarray(gla_norm_g[l], f32)[None]
            pbc[:, 128:512] = np.concatenate([mu[0:512][hs], mu[512:1024][hs], mu[1024:1536][hs]])[None]
            pbc[:, 512:640] = np.asarray(rwkv_k_k[l], f32)[hs][None]
            pbc[:, 640:768] = np.asarray(rwkv_k_a[l], f32)[hs][None]
            pbc[:, 768:896] = np.asarray(rwkv_gn_g[l], f32)[hs][None]
            pbc[:, 896:1024] = np.asarray(rwkv_gn_b[l], f32)[hs][None]
            pbc[:, 1024:1152] = np.asarray(rwkv_r_k[l], f32).reshape(-1)[hs][None]
            ppp = np.zeros((128, 3), f32)
            ppp[0:64, 0] = mu[1536:1600]
            ppp[0:64, 1] = mu[1600:1664]
            ppp[:, 2] = mu[1664:1792]
            wsm = np.zeros((128, 4, 128), f32)
            wsm[0:16, 0, 0:64] = np.asarray(gla_w_alpha[l], f32)[:, 64 * g:64 * g + 64]
            wsm[0:64, 1] = np.asarray(rwkv_w_up[l], f32)[:, hs]
            wsm[0:64, 2] = np.asarray(rwkv_a_up[l], f32)[:, hs]
            wsm[:, 3] = np.asarray(rwkv_g_up[l], f32)[:, hs]
            maps.append({"uT": uT_full[b], "wfm": np.ascontiguousarray(w_in[l][:, fm]),
                         "wtm": np.ascontiguousarray(w_in[l][:, tm]), "cst": cst,
                         "attb": make_att_bias(g).reshape(128, -1), "prow": prow, "pbc": pbc, "ppp": ppp,
                         "wsm": wsm.reshape(128, -1)})
        res = run_bass_kernel_spmd(nc_mix, maps, core_ids=cores).results
        outT = [r["outT"] for r in res]
        wg = np.ascontiguousarray(w_in[l][:, GATE_OFF:])
        maps = []
        for i in cores:
            b, q = divmod(i, 4)
            tsl = slice(q * NTOK, (q + 1) * NTOK)
            brT = np.concatenate([outT[4 * b + g][n * 128:(n + 1) * 128, tsl] for n in range(3) for g in range(4)], axis=0)
            bcv = np.stack([_bcrow(mv(l, b, 2)), _bcrow(np.asarray(ln1_g[l], f32)), _bcrow(np.asarray(ln1_b[l], f32)),
                            _bcrow(mv(l, b, 5)), _bcrow(np.asarray(ln2_g[l], f32)), _bcrow(np.asarray(ln2_b[l], f32))])
            maps.append({"x": np.ascontiguousarray(x_sh[i]), "uT": np.ascontiguousarray(uT_sh[i]),
                         "brT": np.ascontiguousarray(brT), "w_gate": wg, "w_branch": A_(w_branch[l]),
                         "w_out": A_(w_out[l]), "w1": A_(ffn_w1[l]), "w2": A_(ffn_w2[l]),
                         "bcv": np.ascontiguousarray(bcv), "modp": modp_for(l, b), "cst": cst})
        res = run_bass_kernel_spmd(nc_dense, maps, core_ids=cores).results
        x_sh = [r["xo"] for r in res]
        uT_sh = [r["uTn"] for r in res]
    out = np.stack([np.concatenate(x_sh[4 * b:4 * b + 4], axis=0) for b in range(BATCH)], axis=0)
    return np.ascontiguousarray(out.astype(f32))
```
